# Optimizing a Trainium2 kernel written in Bass

```python
import math
import jax
import jax.numpy as jnp
from jax import lax
import numpy as np

D_MODEL = 1024
BATCH = 8
SEQ = 2048
DEPTH = 2

GRID_W = 64
CTX_LEN = 256

DN_HEADS = 4
DN_HEAD_DIM = D_MODEL // 8
DN_WIDTH = DN_HEADS * DN_HEAD_DIM
POOL_WINDOWS = (2, 4, 8, 16)
POOL_GROUPS = len(POOL_WINDOWS)
POOL_WIDTH = D_MODEL // 4
POOL_GROUP_DIM = POOL_WIDTH // POOL_GROUPS
FNET_GROUPS = 4
FNET_WIDTH = D_MODEL - DN_WIDTH - POOL_WIDTH
FNET_GROUP_DIM = FNET_WIDTH // FNET_GROUPS
CONV_WIDTH = 4
CHUNK = 64

COL_QKV = 3 * DN_WIDTH
COL_BETA = COL_QKV + 2 * DN_HEADS
COL_ALPHA = COL_BETA + 2 * DN_HEADS
COL_Z = COL_ALPHA + DN_WIDTH
COL_POOL = COL_Z + POOL_WIDTH
COL_FNET = COL_POOL + FNET_WIDTH
IN_WIDTH = COL_FNET

PEER_HEADS = 8
N_KEYS = 128
N_EXPERTS = N_KEYS * N_KEYS
PEER_TOPK = 16
PEER_KEY_DIM = 256
PEER_KEY_HALF = PEER_KEY_DIM // 2
PEER_BLOCK = 128

N_MOD = 6
RMS_EPS = 1e-6
L2_EPS = 1e-6

kernel_name = 'hybrid_deltanet_pool_fnet_peer_dit'


def _rmsnorm(x, w):
    xf = x.astype(jnp.float32)
    y = xf * lax.rsqrt(jnp.mean(xf * xf, axis=-1, keepdims=True) + RMS_EPS)
    return (y * w.astype(jnp.float32)).astype(x.dtype)


def _l2norm(x):
    return x * lax.rsqrt(jnp.sum(x * x, axis=-1, keepdims=True) + L2_EPS)


def _modulate(h, shift, scale):
    return h * (1 + scale) + shift


def _short_conv(x, w):
    left = CONV_WIDTH // 2
    right = CONV_WIDTH - 1 - left
    return lax.conv_general_dilated(x, w[:, None, :].astype(x.dtype), window_strides=(1,),
                                    padding=[(left, right)],
                                    dimension_numbers=('NWC', 'WIO', 'NWC'),
                                    feature_group_count=x.shape[-1])


def _delta_prep(p_qkv, p_beta, p_alpha, conv_w, a_log, dt_bias):
    b, l, _ = p_qkv.shape
    qkv = jax.nn.silu(_short_conv(p_qkv, conv_w)).astype(jnp.float32)
    qkv = qkv.reshape(b, l, 3, DN_HEADS, DN_HEAD_DIM).transpose(2, 0, 3, 1, 4)
    q = _l2norm(qkv[0]) * (DN_HEAD_DIM ** -0.5)
    k = _l2norm(qkv[1])
    v = qkv[2]
    beta = jax.nn.sigmoid(p_beta.astype(jnp.float32)).reshape(b, l, 2, DN_HEADS).transpose(2, 0, 3, 1)
    a = p_alpha.astype(jnp.float32).reshape(b, l, 2, DN_HEADS).transpose(2, 0, 3, 1)
    g = -jnp.exp(a_log.astype(jnp.float32))[:, None, :, None] * jax.nn.softplus(
        a + dt_bias.astype(jnp.float32)[:, None, :, None])
    return q, k, v, g, beta


def _gated_delta_chunked(q, k, v, g, beta, s0):
    b, h, l, _ = q.shape
    dv = v.shape[-1]
    n = l // CHUNK
    q, k, v = (t.reshape(b, h, n, CHUNK, t.shape[-1]) for t in (q, k, v))
    g = jnp.cumsum(g.reshape(b, h, n, CHUNK), axis=-1)
    beta = beta.reshape(b, h, n, CHUNK)
    incl = jnp.tril(jnp.ones((CHUNK, CHUNK), dtype=bool))
    strict = jnp.tril(jnp.ones((CHUNK, CHUNK), dtype=bool), -1)
    decay = jnp.exp(jnp.where(incl, g[..., :, None] - g[..., None, :], -jnp.inf))
    k_beta = k * beta[..., None]
    lower = jnp.where(strict, jnp.einsum('bhnid,bhnjd->bhnij', k_beta, k) * decay, 0.0)
    a_mat = jnp.eye(CHUNK, dtype=jnp.float32) + lower
    rhs = jnp.concatenate([v * beta[..., None], k_beta * jnp.exp(g)[..., None]], axis=-1)
    sol = lax.linalg.triangular_solve(a_mat, rhs, left_side=True, lower=True, unit_diagonal=True)
    u, w = sol[..., :dv], sol[..., dv:]
    intra = jnp.where(incl, jnp.einsum('bhnid,bhnjd->bhnij', q, k) * decay, 0.0)
    q_decay = q * jnp.exp(g)[..., None]
    k_tail = k * jnp.exp(g[..., -1:] - g)[..., None]
    chunk_decay = jnp.exp(g[..., -1])
    xs = tuple(jnp.moveaxis(t, 2, 0) for t in (q_decay, k_tail, u, w, intra, chunk_decay))

    def step(s, inp):
        qd, kt, uc, wc, ac, dc = inp
        v_new = uc - jnp.einsum('bhcd,bhde->bhce', wc, s)
        o = jnp.einsum('bhcd,bhde->bhce', qd, s) + jnp.einsum('bhcj,bhje->bhce', ac, v_new)
        s = s * dc[..., None, None] + jnp.einsum('bhcd,bhce->bhde', kt, v_new)
        return s, o

    s_fin, o = lax.scan(step, s0, xs)
    return jnp.moveaxis(o, 0, 2).reshape(b, h, l, dv), s_fin


def _rev(t, on):
    return jnp.flip(t, axis=2) if on else t


def _bidir_delta(lat, ctx):
    ql, kl, vl, gl, bl = lat
    qc, kc, vc, gc, bc = ctx
    s0 = jnp.zeros((ql.shape[0], DN_HEADS, DN_HEAD_DIM, DN_HEAD_DIM), jnp.float32)
    o_lat = 0.0
    o_ctx = 0.0
    for d in range(2):
        rv = d == 1
        oc, sc = _gated_delta_chunked(_rev(qc, rv), _rev(kc, rv), _rev(vc, rv),
                                      _rev(gc[d], rv), _rev(bc[d], rv), s0)
        ol, _ = _gated_delta_chunked(_rev(ql, rv), _rev(kl, rv), _rev(vl, rv),
                                     _rev(gl[d], rv), _rev(bl[d], rv), sc)
        o_lat = o_lat + _rev(ol, rv)
        o_ctx = o_ctx + _rev(oc, rv)
    return o_lat, o_ctx


def _dn_output(o, zg, dn_norm_w):
    b, h, l, dh = o.shape
    o = o.transpose(0, 2, 1, 3)
    o = o * lax.rsqrt(jnp.mean(o * o, axis=-1, keepdims=True) + RMS_EPS) * dn_norm_w.astype(jnp.float32)
    o = o * jax.nn.silu(zg.astype(jnp.float32)).reshape(b, l, h, dh)
    return o.reshape(b, l, DN_WIDTH).astype(zg.dtype)


def _centred_window_mean(x, window):
    s_len = x.shape[2]
    cs = jnp.cumsum(x, axis=2)
    cs = jnp.concatenate([jnp.zeros_like(cs[:, :, :1]), cs], axis=2)
    pos = jnp.arange(s_len)
    lo = jnp.clip(pos - window // 2, 0, s_len)
    hi = jnp.clip(pos + window - window // 2, 0, s_len)
    total = jnp.take(cs, hi, axis=2) - jnp.take(cs, lo, axis=2)
    return total / (hi - lo).astype(x.dtype)[:, None]


def _pool_mix(p, w_pool, pool_scale, rows):
    b, l, _ = p.shape
    pg = p.astype(jnp.float32).reshape(b, rows, l // rows, POOL_GROUPS, POOL_GROUP_DIM)
    y = jnp.stack([_centred_window_mean(pg[..., i, :], w) - pg[..., i, :]
                   for i, w in enumerate(POOL_WINDOWS)], axis=-2)
    y = jnp.einsum('brsgc,gcd->brsgd', y, w_pool.astype(jnp.float32))
    return (y.reshape(b, l, POOL_WIDTH) * pool_scale.astype(jnp.float32)).astype(p.dtype)


def _fourier_mix(p, w_fnet):
    b, l, _ = p.shape
    pg = p.astype(jnp.float32).reshape(b, l, FNET_GROUPS, FNET_GROUP_DIM)
    y = jnp.fft.fft2(pg, axes=(1, 3), norm='ortho').real
    y = jnp.einsum('blgc,gcd->blgd', y, w_fnet.astype(jnp.float32))
    return y.reshape(b, l, FNET_WIDTH).astype(p.dtype)


def _mixer_out(p, o_dn, rows, dn_norm_w, w_pool, pool_scale, w_fnet, w_out):
    dn = _dn_output(o_dn, p[..., COL_ALPHA:COL_Z], dn_norm_w)
    pool = _pool_mix(p[..., COL_Z:COL_POOL], w_pool, pool_scale, rows)
    fnet = _fourier_mix(p[..., COL_POOL:COL_FNET], w_fnet)
    return jnp.concatenate([dn, pool, fnet], axis=-1) @ w_out


def _peer(h, w_query, sub_keys, expert_u, expert_v):
    t, d = h.shape
    q = (h @ w_query).astype(jnp.float32).reshape(t, PEER_HEADS, 2, PEER_KEY_HALF)
    scores = jnp.einsum('thpc,hpnc->thpn', q, sub_keys.astype(jnp.float32))
    s_top, i_top = lax.top_k(scores, PEER_TOPK)
    cand = s_top[:, :, 0, :, None] + s_top[:, :, 1, None, :]
    cand_idx = i_top[:, :, 0, :, None] * N_KEYS + i_top[:, :, 1, None, :]
    best, pos = lax.top_k(cand.reshape(t, PEER_HEADS, PEER_TOPK * PEER_TOPK), PEER_TOPK)
    idx = jnp.take_along_axis(cand_idx.reshape(t, PEER_HEADS, PEER_TOPK * PEER_TOPK), pos, axis=-1)
    gate = jax.nn.softmax(best, axis=-1)
    n_blk = t // PEER_BLOCK

    def retrieve(args):
        hb, ib, gb = args
        act = jax.nn.gelu(jnp.einsum('td,thkd->thk', hb, expert_u[ib]).astype(jnp.float32),
                          approximate=False)
        wgt = (gb * act).astype(hb.dtype)
        return jnp.einsum('thk,thkd->td', wgt, expert_v[ib])

    out = lax.map(retrieve, (h.reshape(n_blk, PEER_BLOCK, d),
                             idx.reshape(n_blk, PEER_BLOCK, PEER_HEADS, PEER_TOPK),
                             gate.reshape(n_blk, PEER_BLOCK, PEER_HEADS, PEER_TOPK)))
    return out.reshape(t, d)


def _layer(x, z, c, c_ctx, rows, update_ctx, w_mod, b_mod, norm1_w, norm2_w, w_in, conv_w,
           a_log, dt_bias, dn_norm_w, w_pool, pool_scale, w_fnet, w_out, w_query, sub_keys,
           expert_u, expert_v):
    b, l, d = x.shape
    m = z.shape[1]
    xs1, xc1, xg1, xs2, xc2, xg2 = jnp.split((jax.nn.silu(c) @ w_mod + b_mod)[:, None, :], N_MOD, axis=-1)
    zs1, zc1, zg1, zs2, zc2, zg2 = jnp.split(jax.nn.silu(c_ctx) @ w_mod + b_mod, N_MOD, axis=-1)

    px = _modulate(_rmsnorm(x, norm1_w), xs1, xc1) @ w_in
    pz = _modulate(_rmsnorm(z, norm1_w), zs1, zc1) @ (w_in if update_ctx else w_in[:, :COL_ALPHA])
    lat = _delta_prep(px[..., :COL_QKV], px[..., COL_QKV:COL_BETA], px[..., COL_BETA:COL_ALPHA],
                      conv_w, a_log, dt_bias)
    ctx_in = _delta_prep(pz[..., :COL_QKV], pz[..., COL_QKV:COL_BETA], pz[..., COL_BETA:COL_ALPHA],
                         conv_w, a_log, dt_bias)
    o_lat, o_ctx = _bidir_delta(lat, ctx_in)
    x = x + xg1 * _mixer_out(px, o_lat, rows, dn_norm_w, w_pool, pool_scale, w_fnet, w_out)
    if update_ctx:
        z = z + zg1 * _mixer_out(pz, o_ctx, 1, dn_norm_w, w_pool, pool_scale, w_fnet, w_out)

    hx = _modulate(_rmsnorm(x, norm2_w), xs2, xc2)
    x = x + xg2 * _peer(hx.reshape(b * l, d), w_query, sub_keys, expert_u, expert_v).reshape(b, l, d)
    if update_ctx:
        hz = _modulate(_rmsnorm(z, norm2_w), zs2, zc2)
        z = z + zg2 * _peer(hz.reshape(b * m, d), w_query, sub_keys, expert_u, expert_v).reshape(b, m, d)
    return x, z


def setup_inputs(seed: int = 0) -> dict:
    key = jax.random.key(seed)
    ks = jax.random.split(key, 24)
    d = D_MODEL

    def nrm(k, shape, scale):
        return jax.random.normal(k, shape, jnp.float32) * scale

    dt = jnp.exp(jax.random.uniform(ks[9], (DEPTH, 2, DN_HEADS), jnp.float32,
                                    minval=math.log(1e-3), maxval=math.log(1e-1)))
    return {
        'x': nrm(ks[0], (BATCH, SEQ, d), 1.0),
        'c': nrm(ks[1], (BATCH, d), 1.0),
        'ctx': nrm(ks[2], (BATCH, CTX_LEN, d), 1.0),
        'c_ctx': nrm(ks[3], (d,), 1.0),
        'w_mod': nrm(ks[4], (DEPTH, d, N_MOD * d), 0.5 * d ** -0.5),
        'b_mod': nrm(ks[5], (DEPTH, N_MOD * d), 0.02),
        'norm1_w': 1.0 + nrm(ks[6], (DEPTH, d), 0.02),
        'norm2_w': 1.0 + nrm(ks[7], (DEPTH, d), 0.02),
        'w_in': nrm(ks[8], (DEPTH, d, IN_WIDTH), d ** -0.5),
        'conv_w': nrm(ks[10], (DEPTH, CONV_WIDTH, COL_QKV), CONV_WIDTH ** -0.5),
        'a_log': jnp.log(jax.random.uniform(ks[11], (DEPTH, 2, DN_HEADS), jnp.float32, minval=1.0, maxval=16.0)),
        'dt_bias': dt + jnp.log(-jnp.expm1(-dt)),
        'dn_norm_w': 1.0 + nrm(ks[12], (DEPTH, DN_HEAD_DIM), 0.02),
        'w_pool': nrm(ks[13], (DEPTH, POOL_GROUPS, POOL_GROUP_DIM, POOL_GROUP_DIM), POOL_GROUP_DIM ** -0.5),
        'pool_scale': 1.0 + nrm(ks[14], (DEPTH, POOL_WIDTH), 0.1),
        'w_fnet': nrm(ks[15], (DEPTH, FNET_GROUPS, FNET_GROUP_DIM, FNET_GROUP_DIM), FNET_GROUP_DIM ** -0.5),
        'w_out': nrm(ks[16], (DEPTH, d, d), d ** -0.5),
        'w_query': nrm(ks[17], (DEPTH, d, PEER_HEADS * PEER_KEY_DIM), d ** -0.5),
        'sub_keys': nrm(ks[18], (DEPTH, PEER_HEADS, 2, N_KEYS, PEER_KEY_HALF), PEER_KEY_HALF ** -0.5),
        'expert_u': nrm(ks[19], (DEPTH, N_EXPERTS, d), d ** -0.5),
        'expert_v': nrm(ks[20], (DEPTH, N_EXPERTS, d), PEER_HEADS ** -0.5),
        'final_norm_w': 1.0 + nrm(ks[21], (d,), 0.02),
    }


def reference(x, c, ctx, c_ctx, w_mod, b_mod, norm1_w, norm2_w, w_in, conv_w, a_log, dt_bias,
              dn_norm_w, w_pool, pool_scale, w_fnet, w_out, w_query, sub_keys, expert_u, expert_v,
              final_norm_w):
    rows = x.shape[1] // GRID_W
    z = ctx
    for i in range(DEPTH):
        x, z = _layer(x, z, c, c_ctx, rows, i < DEPTH - 1, w_mod[i], b_mod[i], norm1_w[i], norm2_w[i],
                      w_in[i], conv_w[i], a_log[i], dt_bias[i], dn_norm_w[i], w_pool[i], pool_scale[i],
                      w_fnet[i], w_out[i], w_query[i], sub_keys[i], expert_u[i], expert_v[i])
    return _rmsnorm(x, final_norm_w)
```

```python
import math
from contextlib import ExitStack

import ml_dtypes
import numpy as np

import concourse.bass as bass
import concourse.mybir as mybir
from concourse.bass_utils import run_bass_kernel_spmd

F32 = mybir.dt.float32
BF16 = mybir.dt.bfloat16
I32 = mybir.dt.int32
U32 = mybir.dt.uint32
AF = mybir.ActivationFunctionType
ALU = mybir.AluOpType
AX = mybir.AxisListType

D_MODEL = 1024
SEQ = 2048
CTX = 256
NT = 18
COL_QKV = 1536
COL_BETA = 1544
COL_ALPHA = 1552
COL_Z = 2064
COL_POOL = 2320
COL_FNET = 2576
N_EXP = 16384
SEQW = 2310


def col0(t):
    return 2 + 128 * t if t < 2 else 261 + 128 * (t - 2)


class Prog:
    ENG = ('pe', 'dve', 'act', 'pool', 'sp')

    def __init__(self, nc, es, ndma=None):
        self.nc = nc
        self.cnt = {e: 0 for e in self.ENG}
        self.seen = {e: {} for e in self.ENG}
        self.pending = {e: [] for e in self.ENG}
        self.sem = {e: es.enter_context(nc.semaphore('s_' + e)) for e in self.ENG}
        ndma = ndma or {'sp': 10, 'pool': 10, 'act': 4}
        self.dsem, self.dcnt, self.drr = {}, {}, {}
        for q, n in ndma.items():
            self.dsem[q] = [es.enter_context(nc.semaphore(f'd_{q}{i}')) for i in range(n)]
            self.dcnt[q] = [0] * n
            self.drr[q] = 0
        self.last_w = {}
        self.readers = {}
        self.gran = {}
        self.excl = set()
        self.eng_obj = {'pe': nc.tensor, 'dve': nc.vector, 'act': nc.scalar, 'pool': nc.gpsimd, 'sp': nc.sync}
        self.ninstr = {e: 0 for e in self.ENG}

    def _keys(self, a):
        if isinstance(a, str):
            return [a]
        name = a.tensor.name
        g = self.gran.get(name)
        if g is None:
            return [name]
        shp = a.tensor.shape
        pstride = 1
        for s in shp[1:]:
            pstride *= s
        off = a.offset % pstride
        ext = 0
        for step, cnt in list(a.ap)[1:]:
            ext += (cnt - 1) * step
        return [(name, i) for i in range(off // g, (off + ext) // g + 1)]

    def _sem_of(self, tok):
        if tok[0] == 'e':
            return self.sem[tok[1]]
        return self.dsem[tok[1]][tok[2]]

    def _wait(self, eng, tok):
        k = tok[:-1]
        v = tok[-1]
        if self.seen[eng].get(k, 0) >= v:
            return
        self.seen[eng][k] = v
        self.pending[eng].append((self._sem_of(tok), v))

    def _flush(self, eng, keep=0):
        pend = self.pending[eng]
        best, order = {}, []
        for sem, v in pend:
            key = id(sem)
            if key not in best:
                order.append((key, sem))
                best[key] = v
            else:
                best[key] = max(best[key], v)
        items = [(sem, best[key]) for key, sem in order]
        self.pending[eng] = []
        n_alone = max(len(items) - keep, 0)
        E = self.eng_obj[eng]
        for sem, v in items[:n_alone]:
            self.ninstr[eng] += 1
            E.wait_ge(sem, v)
        return items[n_alone:]

    def _deps(self, eng, r, w, nosync=()):
        nos = set()
        for a in nosync:
            nos.update(self._keys(a))
        toks = []
        for a in r:
            for k in self._keys(a):
                t = self.last_w.get(k)
                if t is not None and not (k in nos and t[0] == 'e' and t[1] == eng):
                    toks.append(t)
                if k in self.excl:
                    for t in self.readers.get(k, ()):
                        if not (t[0] == 'e' and t[1] == eng):
                            toks.append(t)
        for a in w:
            for k in self._keys(a):
                t = self.last_w.get(k)
                if t is not None and not (k in nos and t[0] == 'e' and t[1] == eng):
                    toks.append(t)
                for t in self.readers.get(k, ()):
                    if not (k in nos and t[0] == 'e' and t[1] == eng):
                        toks.append(t)
        for t in toks:
            self._wait(eng, t)

    def _commit(self, tok, r, w):
        wk = set()
        for a in w:
            for k in self._keys(a):
                wk.add(k)
                self.last_w[k] = tok
                self.readers[k] = []
        for a in r:
            for k in self._keys(a):
                if k in wk:
                    continue
                lst = self.readers.setdefault(k, [])
                lst.append(tok)
                if len(lst) > 16:
                    best = {}
                    for t in lst:
                        kk = t[:-1]
                        if best.get(kk, 0) < t[-1]:
                            best[kk] = t[-1]
                    self.readers[k] = [kk + (v,) for kk, v in best.items()]

    def op(self, eng, fn, r=(), w=(), nosync=()):
        self._deps(eng, r, w, nosync)
        last = self._flush(eng, keep=1)
        self.cnt[eng] += 1
        ins = fn(self.eng_obj[eng])
        if last:
            ins = ins._wait_ge(last[0][0], last[0][1])
        ins.then_inc(self.sem[eng], 1)
        self.ninstr[eng] += 1
        self._commit(('e', eng, self.cnt[eng]), r, w)

    def dma(self, q, fn, r=(), w=()):
        self._deps(q, r, w)
        i = self.drr[q]
        self.drr[q] = (i + 1) % len(self.dsem[q])
        prev = self.dcnt[q][i]
        if prev:
            self._wait(q, ('d', q, i, prev))
        last = self._flush(q, keep=1)
        self.dcnt[q][i] = prev + 16
        ins = fn(self.eng_obj[q])
        if last:
            ins = ins._wait_ge(last[0][0], last[0][1])
        ins.then_inc(self.dsem[q][i], 16)
        self.ninstr[q] += 1
        self._commit(('d', q, i, prev + 16), r, w)

    def barrier(self):
        toks = [('e', e, self.cnt[e]) for e in self.ENG if self.cnt[e]]
        for q in self.dsem:
            for i, v in enumerate(self.dcnt[q]):
                if v:
                    toks.append(('d', q, i, v))
        for e in self.ENG:
            for t in toks:
                self._wait(e, t)
            self._flush(e, keep=0)
        self.last_w = {}
        self.readers = {}

    def mm(self, out, lhsT, rhs, start=True, stop=True):
        r = [lhsT, rhs]
        self.op('pe', lambda E: E.matmul(out, lhsT, rhs, start=start, stop=stop), r=r, w=[out], nosync=[out])

    def tr(self, out, in_, ident):
        self.op('pe', lambda E: E.transpose(out, in_, ident), r=[in_, ident], w=[out], nosync=[out])

    def act(self, out, in_, func, bias=None, scale=None, accum_out=None):
        kw = {}
        r, w = [in_], [out]
        if bias is not None:
            kw['bias'] = bias
            if not isinstance(bias, (int, float)):
                r.append(bias)
        if scale is not None:
            kw['scale'] = scale
            if not isinstance(scale, (int, float)):
                r.append(scale)
        if accum_out is not None:
            kw['accum_out'] = accum_out
            w.append(accum_out)
        self.op('act', lambda E: E.activation(out, in_, func, **kw), r=r, w=w)

    def tt(self, eng, out, in0, in1, op):
        self.op(eng, lambda E: E.tensor_tensor(out, in0, in1, op), r=[in0, in1], w=[out])

    def ts(self, eng, out, in0, s1, op0, s2=None, op1=None):
        r, w = [in0], [out]
        if not isinstance(s1, (int, float)):
            r.append(s1)
        if s2 is not None and not isinstance(s2, (int, float)):
            r.append(s2)
        kw = {}
        if op1 is not None:
            kw['op1'] = op1
        self.op(eng, lambda E: E.tensor_scalar(out, in0, s1, s2, op0, **kw), r=r, w=w)

    def stt(self, eng, out, in0, scalar, in1, op0, op1, accum_out=None):
        r, w = [in0, in1], [out]
        if not isinstance(scalar, (int, float)):
            r.append(scalar)
        kw = {}
        if accum_out is not None:
            kw['accum_out'] = accum_out
            w.append(accum_out)
        self.op(eng, lambda E: E.scalar_tensor_tensor(out, in0, scalar, in1, op0, op1, **kw), r=r, w=w)

    def copy(self, eng, out, in_):
        if eng == 'act':
            self.op('act', lambda E: E.copy(out, in_), r=[in_], w=[out])
        else:
            self.op(eng, lambda E: E.tensor_copy(out, in_), r=[in_], w=[out])

    def memset(self, eng, out, val):
        self.op(eng, lambda E: E.memset(out, val), r=[], w=[out])

    def load(self, q, out, in_):
        self.dma(q, lambda E: E.dma_start(out=out, in_=in_), r=[in_], w=[out])


def _pool_matrix(seg, window):
    m = np.zeros((seg, seg), np.float64)
    for s in range(seg):
        lo = min(max(s - window // 2, 0), seg)
        hi = min(max(s + window - window // 2, 0), seg)
        m[s, lo:hi] = 1.0 / (hi - lo)
    return m


def make_consts():
    c = {}
    c['ident'] = np.eye(128, dtype=np.float32)
    zc = np.zeros((128, 255), np.float32)
    zc[:, 127] = 1.0
    c['zc'] = zc
    i = np.arange(128)[:, None]
    j = np.arange(128)[None, :]
    same = (i // 64) == (j // 64)
    msk = np.zeros((128, 4, 128), np.float32)
    msk[:, 0, :] = same & (j < i)
    msk[:, 1, :] = same & (j > i)
    msk[:, 2, :] = same & (i <= j)
    msk[:, 3, :] = same & (i >= j)
    c['masks'] = msk
    tri = np.zeros((128, 2, 130), np.float32)
    tri[:, 0, :128] = same & (i <= j)
    tri[:, 1, :128] = same & (i >= j)
    tri[:64, :, 128] = 1.0
    tri[64:, :, 129] = 1.0
    c['trix'] = tri
    c['blk'] = same.astype(np.float32)
    wins = (2, 4, 8, 16)
    pml = np.zeros((128, 4, 128), np.float32)
    for g, w in enumerate(wins):
        m = _pool_matrix(64, w) - np.eye(64)
        bd = np.zeros((128, 128))
        bd[:64, :64] = m
        bd[64:, 64:] = m
        pml[:, g, :] = bd.T
    c['pm_lat'] = pml
    pmc = np.zeros((128, 2, 4, 256), np.float32)
    for g, w in enumerate(wins):
        m = (_pool_matrix(256, w) - np.eye(256)).T
        for jt in range(2):
            pmc[:, jt, g, :] = m[jt * 128:(jt + 1) * 128, :]
    c['pm_ctx'] = pmc
    k = np.arange(64)
    ang = 2 * np.pi * np.outer(k, k) / 64
    c64 = np.zeros((128, 128), np.float32)
    s64 = np.zeros((128, 128), np.float32)
    for b in range(2):
        c64[b * 64:(b + 1) * 64, b * 64:(b + 1) * 64] = np.cos(ang)
        s64[b * 64:(b + 1) * 64, b * 64:(b + 1) * 64] = -np.sin(ang)
    c['c64bd'] = c64
    c['s64bdn'] = s64
    for name, L in (('', SEQ), ('256', CTX)):
        n = np.arange(L)
        prod = np.outer(n, n) % L
        a = 2 * np.pi * prod / L
        sc = 1.0 / math.sqrt(L * 64)
        c['dftc' + name] = (np.cos(a) * sc).astype(ml_dtypes.bfloat16)
        c['dfts' + name] = (np.sin(a) * sc).astype(ml_dtypes.bfloat16)
    c['iota16'] = np.broadcast_to(np.arange(16, dtype=np.float32), (128, 16)).copy()
    return c


def prep_shared(inp):
    s = {}
    f = lambda a: np.ascontiguousarray(a, dtype=np.float32)
    s['w_mod'] = f(inp['w_mod'])
    s['b_mod'] = f(inp['b_mod'])
    s['bmod_pp'] = f(inp['b_mod'][:, :2048].reshape(2, 16, 128).transpose(0, 2, 1))
    s['norm1_pp'] = f(inp['norm1_w'].reshape(2, 8, 128).transpose(0, 2, 1))
    s['norm2_w'] = f(inp['norm2_w'])
    s['final_norm_w'] = f(inp['final_norm_w'].reshape(1, 1024))
    s['w_in'] = f(inp['w_in'])
    s['convw_pp'] = f(inp['conv_w'].transpose(0, 2, 1).reshape(2, 12, 128, 4).transpose(0, 2, 1, 3))
    s['alog_rep'] = f(np.broadcast_to(inp['a_log'].reshape(2, 1, 8), (2, 128, 8)))
    s['dtb_rep'] = f(np.broadcast_to(inp['dt_bias'].reshape(2, 1, 8), (2, 128, 8)))
    s['dnw_rep'] = f(np.broadcast_to(inp['dn_norm_w'].reshape(2, 1, 128), (2, 128, 128)))
    s['wpool_l'] = f(inp['w_pool'].transpose(0, 2, 1, 3))
    s['pscale_pp'] = f(inp['pool_scale'].reshape(2, 4, 64).transpose(0, 2, 1))
    s['w_fnet'] = f(inp['w_fnet'])
    s['w_out'] = f(inp['w_out'])
    s['w_query'] = f(inp['w_query'])
    s['keysT'] = f(inp['sub_keys'].transpose(0, 4, 1, 2, 3).reshape(2, 128, 16, 128))
    for l in range(2):
        s[f'eu{l}'] = f(inp['expert_u'][l])
        s[f'ev{l}'] = f(inp['expert_v'][l])
    s.update(make_consts())
    return s


_SHAPES = {
    'x': ([SEQ, 1024], F32), 'ctx': ([CTX, 1024], F32), 'cvec': ([128, 8, 2], F32),
    'w_mod': ([2, 1024, 6144], F32), 'b_mod': ([2, 6144], F32), 'bmod_pp': ([2, 128, 16], F32),
    'norm1_pp': ([2, 128, 8], F32), 'norm2_w': ([2, 1024], F32), 'final_norm_w': ([1, 1024], F32),
    'w_in': ([2, 1024, COL_FNET], F32), 'convw_pp': ([2, 128, 12, 4], F32),
    'alog_rep': ([2, 128, 8], F32), 'dtb_rep': ([2, 128, 8], F32), 'dnw_rep': ([2, 128, 128], F32),
    'wpool_l': ([2, 64, 4, 64], F32), 'pscale_pp': ([2, 64, 4], F32), 'w_fnet': ([2, 4, 64, 64], F32),
    'w_out': ([2, 1024, 1024], F32), 'w_query': ([2, 1024, 2048], F32), 'keysT': ([2, 128, 16, 128], F32),
    'eu0': ([N_EXP, 1024], F32), 'eu1': ([N_EXP, 1024], F32), 'ev0': ([N_EXP, 1024], F32), 'ev1': ([N_EXP, 1024], F32),
    'ident': ([128, 128], F32), 'zc': ([128, 255], F32), 'masks': ([128, 4, 128], F32), 'trix': ([128, 2, 130], F32),
    'blk': ([128, 128], F32), 'pm_lat': ([128, 4, 128], F32), 'pm_ctx': ([128, 2, 4, 256], F32),
    'c64bd': ([128, 128], F32), 's64bdn': ([128, 128], F32),
    'dftc': ([SEQ, SEQ], BF16), 'dfts': ([SEQ, SEQ], BF16), 'dftc256': ([CTX, CTX], BF16), 'dfts256': ([CTX, CTX], BF16),
    'iota16': ([128, 16], F32),
}


def build(opts=None):
    opts = opts or {}
    taps_req = opts.get('taps', ())
    stop_after = opts.get('stop_after', None)
    layers = opts.get('layers', (0, 1))
    nc = bass.Bass("TRN2", target_bir_lowering=False)
    D = {}
    for name, (shape, dt) in _SHAPES.items():
        D[name] = nc.dram_tensor(name, shape, dt, kind="ExternalInput").ap()
    out_d = nc.dram_tensor("out", [SEQ, 1024], F32, kind="ExternalOutput").ap()
    tap_d = {}

    with ExitStack() as es:
        P = Prog(nc, es)

        uniq = {'n': 0}

        def sb(stack, name, shape, dt=F32):
            uniq['n'] += 1
            return stack.enter_context(nc.sbuf_tensor(f"s{uniq['n']}_{name}", shape, dt))

        def tap(name, ap, shape, force=False):
            if name not in taps_req and not force:
                return
            t = nc.dram_tensor("tap_" + name, list(shape), F32, kind="ExternalOutput").ap()
            tap_d[name] = t
            P.load('sp', t, ap)

        PS2 = [es.enter_context(nc.psum_tensor(f"pq{j}", [128, 1024], F32)) for j in range(3)]
        PSB = [PS2[i // 2][:, (i % 2) * 512:(i % 2 + 1) * 512] for i in range(6)]
        PTB = [es.enter_context(nc.psum_tensor(f"ptb{i}", [128, 1024], BF16)) for i in range(2)]
        for j in range(3):
            P.gran[f"pq{j}"] = 512
            P.excl.add((f"pq{j}", 0))
            P.excl.add((f"pq{j}", 1))
        for i in range(2):
            P.excl.add(f"ptb{i}")

        XR = [sb(es, f"xr{t}", [128, 1024]) for t in range(NT)]
        ident = sb(es, "ident_s", [128, 128])
        identb = sb(es, "identb_s", [128, 128], BF16)
        csil = sb(es, "csil", [128, 8, 2])
        ss = sb(es, "ss", [128, NT])
        rs = sb(es, "rs", [128, NT])
        tmpr = [sb(es, f"tmpr{i}", [128, 1024]) for i in range(1)]
        rr_state = {'tmpr': 0}

        P.load('sp', ident[:], D['ident'])
        P.copy('dve', identb[:], ident[:])
        for t in range(NT):
            src = D['ctx'][t * 128:(t + 1) * 128, :] if t < 2 else D['x'][(t - 2) * 128:(t - 1) * 128, :]
            P.load('sp', XR[t][:], src)
        P.load('sp', csil[:], D['cvec'])
        P.act(csil[:], csil[:], AF.Silu)

        def rstd_all(tiles, junk):
            for t in tiles:
                P.act(junk[:], XR[t][:], AF.Square, accum_out=ss[:, t:t + 1])
            P.ts('dve', rs[:], ss[:], 1.0 / 1024, ALU.mult, 1e-6, ALU.add)
            P.act(rs[:], rs[:], AF.Ln)
            P.act(rs[:], rs[:], AF.Exp, scale=-0.5)

        def residual_add(t, ps_list, grep):
            tm = tmpr[0]
            rr_state['tmpr'] += 1
            for n in range(2):
                P.tt('dve', tm[:, n * 512:(n + 1) * 512], ps_list[n], grep[:, n * 512:(n + 1) * 512], ALU.mult)
            P.tt('pool', XR[t][:], XR[t][:], tm[:], ALU.add)

        def mod_rep_block(l, which, cbk, dst, wm_t, brow_t, psb, cbc):
            wv = D['w_mod'][l].rearrange("(k p) n -> p k n", p=128)
            P.load('sp', wm_t[:], wv[:, :, cbk * 512:(cbk + 1) * 512])
            P.load('sp', brow_t[:], D['b_mod'][l:l + 1, cbk * 512:(cbk + 1) * 512].to_broadcast([128, 512]))
            for k in range(8):
                P.mm(psb, cbc[which][:, k, :], wm_t[:, k, :], start=(k == 0), stop=(k == 7))
            P.tt('dve', dst, psb, brow_t[:], ALU.add)

        def make_cbc(stack):
            cbc = [sb(stack, f"cbc{w}", [128, 8, 128]) for w in range(2)]
            for w in range(2):
                P.copy('pool', cbc[w][:], csil[:, :, w:w + 1].to_broadcast([128, 8, 128]))
            return cbc

        def load_win(l, c0, n, dst):
            P.load('pool', dst, D['w_in'][l].rearrange("(k p) n -> p k n", p=128)[:, :, c0:c0 + n])

        done = {'stop': False}

        EB = {}
        CVH = {}

        def conv_gen(l):
            dst = nc.dram_tensor(f"euvb{l}", [N_EXP, 2, 1024], BF16).ap()
            EB[l] = dst.rearrange("e two n -> e (two n)")
            d4 = dst.rearrange("(p r) two n -> p r two n", p=128)
            ci = 0
            for half, nm in enumerate(('eu', 'ev')):
                sv = D[f"{nm}{l}"].rearrange("(p r) n -> p (r n)", p=128)
                for c in range(32):
                    b_ = CVH['cvb'][ci % 2]
                    ci += 1
                    P.load('pool', b_[:], sv[:, c * 4096:(c + 1) * 4096])
                    P.load('sp', d4[:, c * 4:(c + 1) * 4, half, :], b_[:].rearrange("p (r n) -> p r n", n=1024))
                    yield

        def conv_pull(n):
            g = CVH.get('gen')
            if g is None:
                return
            for _ in range(n):
                try:
                    next(g)
                except StopIteration:
                    CVH['gen'] = None
                    return

        def check_stop(tag):
            if stop_after == tag:
                done['stop'] = True
            return done['stop']

        for l in layers:
            upd = (l == 0)
            tiles_all = list(range(NT))
            tiles_upd = tiles_all if upd else list(range(2, NT))
            if not opts.get('skip_mix'):
                with ExitStack() as lay:
                    a1 = sb(lay, "a1", [128, 8, 2])
                    s1 = sb(lay, "s1", [128, 8, 2])
                    g1rep = [sb(lay, f"g1rep{w}", [128, 1024]) for w in range(2)]
                    xnT = sb(lay, "xnT", [128, 8, NT * 128], BF16)
                    cvs = ExitStack()
                    CVH['cvb'] = [sb(cvs, f"cvb{i}", [128, 4096], BF16) for i in range(2)]
                    CVH['gen'] = conv_gen(l)

                    with ExitStack() as tmp:
                        wm = [sb(tmp, f"wm{i}", [128, 8, 512]) for i in range(2)]
                        brow = [sb(tmp, f"brow{i}", [128, 512]) for i in range(2)]
                        bpp = sb(tmp, "bpp", [128, 16])
                        n1 = sb(tmp, "n1pp", [128, 8])
                        pp = sb(tmp, "pp", [128, 16, 2])
                        cbc = make_cbc(tmp)
                        P.load('sp', bpp[:], D['bmod_pp'][l])
                        P.load('sp', n1[:], D['norm1_pp'][l])
                        wv = D['w_mod'][l].rearrange("(k p) n -> p k n", p=128)
                        pp_ps = PSB[0][:, 0:32].rearrange("p (j w) -> p j w", w=2)
                        for jb in range(4):
                            conv_pull(1)
                            w_ = wm[jb % 2]
                            P.load('sp', w_[:], wv[:, :, jb * 512:(jb + 1) * 512])
                            for jj in range(4):
                                j = jb * 4 + jj
                                for k in range(8):
                                    P.mm(pp_ps[:, j, :], w_[:, k, jj * 128:(jj + 1) * 128], csil[:, k, :],
                                         start=(k == 0), stop=(k == 7))
                        P.tt('dve', pp[:], pp_ps, bpp[:].unsqueeze(2).to_broadcast([128, 16, 2]), ALU.add)
                        P.copy('dve', s1[:], pp[:, 0:8, :])
                        P.stt('dve', a1[:], pp[:, 8:16, :], 1.0, n1[:].unsqueeze(2).to_broadcast([128, 8, 2]),
                              ALU.add, ALU.mult)
                        i = 0
                        for which in ((0, 1) if upd else (0,)):
                            for cb in range(2):
                                conv_pull(1)
                                mod_rep_block(l, which, 4 + cb, g1rep[which][:, cb * 512:(cb + 1) * 512], wm[i % 2],
                                              brow[i % 2], PSB[1 + i % 2][:], cbc)
                                i += 1
                        tap(f"L{l}_a1", a1[:].rearrange("p a b -> p (a b)"), [128, 16])
                        tap(f"L{l}_s1", s1[:].rearrange("p a b -> p (a b)"), [128, 16])
                        tap(f"L{l}_g1x", g1rep[0][:], [128, 1024])
                        P.barrier()
                    if check_stop(f"mod{l}"):
                        break

                    with ExitStack() as tmp:
                        junk = sb(tmp, "junkb", [128, 1024], BF16)
                        xsb = [sb(tmp, f"xsb{i}", [128, 1024], BF16) for i in range(2)]
                        rstd_all(tiles_all, junk)
                        for t in tiles_all:
                            conv_pull(2)
                            xs_ = xsb[t % 2]
                            wz = 1 if t < 2 else 0
                            P.ts('dve', xs_[:], XR[t][:], rs[:, t:t + 1], ALU.mult)
                            for half in range(2):
                                PTb = PTB[half]
                                for kk in range(4):
                                    k = half * 4 + kk
                                    P.tr(PTb[:, kk * 256:kk * 256 + 128], xs_[:, k * 128:(k + 1) * 128], identb[:])
                                for kk in range(4):
                                    k = half * 4 + kk
                                    P.act(xnT[:, k, t * 128:(t + 1) * 128], PTb[:, kk * 256:kk * 256 + 128], AF.Identity,
                                          scale=a1[:, k, wz:wz + 1], bias=s1[:, k, wz:wz + 1])
                        if f"L{l}_xnT" in taps_req:
                            xf = sb(tmp, "xnT_f", [128, 8, 512])
                            P.copy('dve', xf[:], xnT[:, :, 0:512])
                            tap(f"L{l}_xnT", xf[:].rearrange("p a b -> p (a b)"), [128, 4096])
                        P.barrier()
                    if check_stop(f"xnT{l}"):
                        break

                    with ExitStack() as tmp:
                        wp = sb(tmp, "wp", [128, 8, 256], BF16)
                        load_win(l, COL_Z, 256, wp[:])
                        wpl = sb(tmp, "wpl", [64, 4, 64])
                        psc = sb(tmp, "psc", [64, 4])
                        pml = sb(tmp, "pml", [128, 4, 128])
                        P.load('sp', wpl[:], D['wpool_l'][l])
                        P.load('sp', psc[:], D['pscale_pp'][l])
                        P.load('sp', pml[:], D['pm_lat'])
                        if upd:
                            pmc = sb(tmp, "pmc", [128, 2, 4, 256])
                            P.load('sp', pmc[:], D['pm_ctx'])
                        wo = sb(tmp, "wo_p", [64, 4, 1024], BF16)
                        P.load('pool', wo[:], D['w_out'][l][512:768, :].rearrange("(g p) n -> p g n", p=64))
                        ptm = [sb(tmp, f"ptm{i}", [128, 256]) for i in range(2)]
                        yT = sb(tmp, "yT", [64, 4, 128])
                        mixp = sb(tmp, "mixp", [64, 4, 128], BF16)
                        psA, psY, psZ = PSB[0], PSB[1], PSB[2]
                        psO = [PSB[3], PSB[4]]

                        def pool_finish(t, which):
                            P.copy('act', yT[:], psY[0:64, :].rearrange("p (g s) -> p g s", g=4))
                            for g in range(4):
                                P.mm(psZ[0:64, g * 128:(g + 1) * 128], wpl[:, g, :], yT[:, g, :])
                            for g in range(4):
                                P.ts('dve', mixp[:, g, :], psZ[0:64, g * 128:(g + 1) * 128], psc[:, g:g + 1], ALU.mult)
                            if f"L{l}_pool" in taps_req and t == 2:
                                mf = sb(tmp, "mixp_f", [64, 4, 128])
                                P.copy('dve', mf[:], mixp[:])
                                tap(f"L{l}_pool", mf[:].rearrange("p a b -> p (a b)"), [64, 512])
                            for n in range(2):
                                for g in range(4):
                                    P.mm(psO[n][:], mixp[:, g, :], wo[:, g, n * 512:(n + 1) * 512],
                                         start=(g == 0), stop=(g == 3))
                            residual_add(t, [psO[0][:], psO[1][:]], g1rep[which])

                        for t in tiles_upd:
                            conv_pull(2)
                            p_ = ptm[t % 2]
                            for k in range(8):
                                P.mm(psA[:, 0:256], xnT[:, k, t * 128:(t + 1) * 128], wp[:, k, :],
                                     start=(k == 0), stop=(k == 7))
                            P.copy('act', p_[:], psA[:, 0:256])
                            if t >= 2:
                                for g in range(4):
                                    P.mm(psY[0:64, g * 128:(g + 1) * 128], p_[:, g * 64:(g + 1) * 64], pml[:, g, :])
                                pool_finish(t, 0)
                            elif t == 1:
                                for it in range(2):
                                    for g in range(4):
                                        P.mm(psY[0:64, g * 128:(g + 1) * 128], ptm[0][:, g * 64:(g + 1) * 64],
                                             pmc[:, 0, g, it * 128:(it + 1) * 128], start=True, stop=False)
                                        P.mm(psY[0:64, g * 128:(g + 1) * 128], ptm[1][:, g * 64:(g + 1) * 64],
                                             pmc[:, 1, g, it * 128:(it + 1) * 128], start=False, stop=True)
                                    pool_finish(it, 1)
                        P.barrier()
                    conv_pull(1000)
                    P.barrier()
                    cvs.close()
                    if check_stop(f"pool{l}"):
                        break

                    with ExitStack() as tmp:
                        wf = sb(tmp, "wf", [128, 8, 256], BF16)
                        load_win(l, COL_POOL, 256, wf[:])
                        wfbd = sb(tmp, "wfbd", [128, 2, 128])
                        P.memset('dve', wfbd[:], 0.0)
                        for g in range(4):
                            P.load('sp', wfbd[(g % 2) * 64:(g % 2 + 1) * 64, g // 2, (g % 2) * 64:(g % 2 + 1) * 64],
                                   D['w_fnet'][l, g])
                        c64 = sb(tmp, "c64", [128, 128])
                        s64 = sb(tmp, "s64", [128, 128])
                        P.load('sp', c64[:], D['c64bd'])
                        P.load('sp', s64[:], D['s64bdn'])
                        Wcs = sb(tmp, "Wcs", [128, 2, 2, 128], BF16)
                        psA = PSB[0]
                        for pr in range(2):
                            P.mm(psA[:, 0:128], c64[:], wfbd[:, pr, :])
                            P.copy('dve', Wcs[:, pr, 0, :], psA[:, 0:128])
                            P.mm(psA[:, 128:256], s64[:], wfbd[:, pr, :])
                            P.copy('dve', Wcs[:, pr, 1, :], psA[:, 128:256])
                        fcut = opts.get('fnet_cut', 99)
                        ABc = sb(tmp, "ABc", [128, NT, 256], BF16)
                        ABs = sb(tmp, "ABs", [128, NT, 256], BF16)
                        pT = [sb(tmp, f"pT{i}", [128, 512], BF16) for i in range(2)]
                        blocks = ([(0, 2)] if upd else []) + [(2 + 4 * b, 4) for b in range(4)]
                        psP = PSB[1]
                        for (t0, ntl) in (blocks if fcut >= 2 else []):
                            for pr in range(2):
                                pt_ = pT[pr]
                                for k in range(8):
                                    P.mm(psP[:, 0:ntl * 128], wf[:, k, pr * 128:(pr + 1) * 128],
                                         xnT[:, k, t0 * 128:(t0 + ntl) * 128], start=(k == 0), stop=(k == 7))
                                P.copy('act', pt_[:, 0:ntl * 128], psP[:, 0:ntl * 128])
                                for s_ in range(ntl):
                                    t = t0 + s_
                                    q = PSB[2 + (s_ % 2)]
                                    P.mm(q[:, 0:128], pt_[:, s_ * 128:(s_ + 1) * 128], Wcs[:, pr, 0, :])
                                    P.mm(q[:, 128:256], pt_[:, s_ * 128:(s_ + 1) * 128], Wcs[:, pr, 1, :])
                                    P.copy('dve', ABc[:, t, pr * 128:(pr + 1) * 128], q[:, 0:128])
                                    P.copy('act', ABs[:, t, pr * 128:(pr + 1) * 128], q[:, 128:256])
                        wo = sb(tmp, "wo_f", [128, 2, 1024], BF16)
                        P.load('pool', wo[:], D['w_out'][l][768:1024, :].rearrange("(f p) n -> p f n", p=128))
                        CL = sb(tmp, "CL", [128, 16, 512], BF16)
                        SL = sb(tmp, "SL", [128, 16, 512], BF16)
                        mixf = sb(tmp, "mixf", [128, 2, 512], BF16)
                        psF = [PSB[4], PSB[5]]
                        psO = [PSB[0], PSB[1]]
                        dc_v = D['dftc'].rearrange("(k p) n -> p k n", p=128)
                        ds_v = D['dfts'].rearrange("(k p) n -> p k n", p=128)
                        for blk in range(4 if fcut >= 5 else (1 if fcut >= 3 else 0)):
                            for q4 in range(4):
                                P.load('sp', CL[:, q4 * 4:(q4 + 1) * 4, :], dc_v[:, q4 * 4:(q4 + 1) * 4, blk * 512:(blk + 1) * 512])
                                P.load('sp', SL[:, q4 * 4:(q4 + 1) * 4, :], ds_v[:, q4 * 4:(q4 + 1) * 4, blk * 512:(blk + 1) * 512])
                            for fc in range(2):
                                for lc in range(16):
                                    P.mm(psF[fc][:], ABc[:, 2 + lc, fc * 128:(fc + 1) * 128], CL[:, lc, :],
                                         start=(lc == 0), stop=False)
                                    P.mm(psF[fc][:], ABs[:, 2 + lc, fc * 128:(fc + 1) * 128], SL[:, lc, :],
                                         start=False, stop=(lc == 15))
                                P.copy('act', mixf[:, fc, :], psF[fc][:])
                            if f"L{l}_fnet" in taps_req and blk == 0:
                                mf = sb(tmp, "mixf_f", [128, 2, 512])
                                P.copy('dve', mf[:], mixf[:])
                                tap(f"L{l}_fnet", mf[:].rearrange("p a b -> p (a b)"), [128, 1024])
                            for s_ in range(4 if fcut >= 4 else 0):
                                t = 2 + blk * 4 + s_
                                for n in range(2):
                                    for fc in range(2):
                                        P.mm(psO[n][:], mixf[:, fc, s_ * 128:(s_ + 1) * 128],
                                             wo[:, fc, n * 512:(n + 1) * 512], start=(fc == 0), stop=(fc == 1))
                                residual_add(t, [psO[0][:], psO[1][:]], g1rep[0])
                        if upd and fcut >= 6:
                            dc2 = D['dftc256'].rearrange("(k p) n -> p k n", p=128)
                            ds2 = D['dfts256'].rearrange("(k p) n -> p k n", p=128)
                            P.load('sp', CL[:, 0:2, 0:256], dc2)
                            P.load('sp', SL[:, 0:2, 0:256], ds2)
                            for fc in range(2):
                                for lc in range(2):
                                    P.mm(psF[fc][:, 0:256], ABc[:, lc, fc * 128:(fc + 1) * 128], CL[:, lc, 0:256],
                                         start=(lc == 0), stop=False)
                                    P.mm(psF[fc][:, 0:256], ABs[:, lc, fc * 128:(fc + 1) * 128], SL[:, lc, 0:256],
                                         start=False, stop=(lc == 1))
                                P.copy('act', mixf[:, fc, 0:256], psF[fc][:, 0:256])
                            for s_ in range(2):
                                for n in range(2):
                                    for fc in range(2):
                                        P.mm(psO[n][:], mixf[:, fc, s_ * 128:(s_ + 1) * 128],
                                             wo[:, fc, n * 512:(n + 1) * 512], start=(fc == 0), stop=(fc == 1))
                                residual_add(s_, [psO[0][:], psO[1][:]], g1rep[1])
                        P.barrier()
                    if check_stop(f"fnet{l}"):
                        break

                    with ExitStack() as tmp:
                        masks = sb(tmp, "masks", [128, 4, 128])
                        trix = sb(tmp, "trix", [128, 2, 130])
                        blk_ = sb(tmp, "blk", [128, 128])
                        ones = sb(tmp, "ones", [128, 128])
                        dnw = sb(tmp, "dnw", [128, 128])
                        cw = sb(tmp, "cw", [128, 12, 4])
                        alog = sb(tmp, "alog", [128, 8])
                        dtb = sb(tmp, "dtb", [128, 8])
                        P.load('sp', masks[:], D['masks'])
                        P.load('sp', trix[:], D['trix'])
                        P.load('sp', blk_[:], D['blk'])
                        P.memset('pool', ones[:], 1.0)
                        P.load('sp', dnw[:], D['dnw_rep'][l])
                        P.load('sp', cw[:], D['convw_pp'][l])
                        P.load('sp', alog[:], D['alog_rep'][l])
                        P.load('sp', dtb[:], D['dtb_rep'][l])
                        wba = sb(tmp, "wba", [128, 8, 16], BF16)
                        load_win(l, COL_QKV, 16, wba[:])
                        sm = lambda n: sb(tmp, n, [128, NT, 8])
                        beta, gtm, Gc, bk, et, xx, ax = (sm(n) for n in ("beta", "gtm", "Gc", "bk", "et", "xx", "ax"))
                        v3 = lambda bank, w: bank[:, 0:NT * w].rearrange("p (t w) -> p t w", w=w)
                        ba_ps = v3(PSB[0], 16)
                        for t in tiles_all:
                            for k in range(8):
                                P.mm(ba_ps[:, t, :], xnT[:, k, t * 128:(t + 1) * 128], wba[:, k, :],
                                     start=(k == 0), stop=(k == 7))
                        P.act(beta[:], ba_ps[:, :, 0:8], AF.Sigmoid)
                        P.tt('dve', xx[:], ba_ps[:, :, 8:16], dtb[:].unsqueeze(1).to_broadcast([128, NT, 8]), ALU.add)
                        P.act(ax[:], xx[:], AF.Abs)
                        P.act(ax[:], ax[:], AF.Exp, scale=-1.0)
                        P.ts('dve', ax[:], ax[:], 1.0, ALU.add)
                        P.act(ax[:], ax[:], AF.Ln)
                        P.ts('dve', xx[:], xx[:], 0.0, ALU.max)
                        P.tt('dve', xx[:], xx[:], ax[:], ALU.add)
                        P.act(alog[:], alog[:], AF.Exp)
                        P.stt('dve', gtm[:], xx[:], -1.0, alog[:].unsqueeze(1).to_broadcast([128, NT, 8]), ALU.mult, ALU.mult)
                        G_ps = v3(PSB[1], 8)
                        Gt_ps = v3(PSB[2], 8)
                        for t in tiles_all:
                            for d in range(2):
                                P.mm(G_ps[:, t, d * 4:(d + 1) * 4], trix[:, d, 0:128], gtm[:, t, d * 4:(d + 1) * 4])
                            P.mm(Gt_ps[:, t, :], blk_[:], gtm[:, t, :])
                        P.copy('dve', Gc[:], G_ps)
                        P.act(bk[:], G_ps, AF.Exp)
                        P.tt('dve', bk[:], bk[:], beta[:], ALU.mult)
                        P.tt('dve', et[:], Gt_ps, Gc[:], ALU.subtract)
                        P.act(et[:], et[:], AF.Exp)
                        tap(f"L{l}_beta", beta[:].rearrange("p a b -> p (a b)"), [128, NT * 8])
                        tap(f"L{l}_gtm", gtm[:].rearrange("p a b -> p (a b)"), [128, NT * 8])

                        pre = sb(tmp, "pre", [128, SEQW])
                        P.gran[pre.name] = 128
                        obuf = pre[:, 0:NT * 128].rearrange("p (t w) -> p t w", w=128)
                        qT = sb(tmp, "qT", [128, SEQW])
                        kT = sb(tmp, "kT", [128, SEQW])
                        vT = sb(tmp, "vT", [128, SEQW])
                        wpc = [sb(tmp, f"wpc{i}", [128, 8, 128], BF16) for i in range(2)]
                        sq = sb(tmp, "sq", [128, 512])
                        rn = sb(tmp, "rn", [128, 512])
                        oss = sb(tmp, "oss", [128, NT])
                        ors = sb(tmp, "ors", [128, NT])
                        wz = sb(tmp, "wz", [128, 8, 128], BF16)
                        woh = sb(tmp, "woh", [128, 1024], BF16)
                        fin = {n: sb(tmp, "fin_" + n, [128, 128]) for n in ("sz", "y1", "junk")}
                        y2 = [sb(tmp, f"fin_y2{i}", [128, 128], BF16) for i in range(2)]
                        mixT = [sb(tmp, f"fin_mixT{i}", [128, 128], BF16) for i in range(2)]
                        WK = []
                        for ch in range(2):
                            names = ("gb", "eG", "E1", "E2", "Pa", "Pb", "Qa", "Qb", "Ta", "Tb", "intraT", "kb", "ktail",
                                     "vb", "u", "wT", "qdT", "vnew", "S")
                            wk = {n: sb(tmp, f"wk{ch}_{n}", [128, 128]) for n in names}
                            wk["dcs"] = sb(tmp, f"wk{ch}_dcs", [128, 2])
                            WK.append(wk)
                        seqblocks = [(0, 256, 2)] + [(256 + b * 512, 512, 261 + b * 512) for b in range(4)]
                        CW = SEQW - 3

                        HN = ("u", "wT", "qdT", "intraT", "ktail")
                        HS = []
                        for ch in range(2):
                            slots = [{n: WK[ch][n] for n in HN + ("dcs",)}]
                            s1_ = {n: sb(tmp, f"hs{ch}_{n}", [128, 128]) for n in HN}
                            s1_["dcs"] = sb(tmp, f"hs{ch}_dcs", [128, 2])
                            slots.append(s1_)
                            HS.append(slots)
                        R32d = [PTB[i][:].bitcast(F32) for i in range(2)]
                        prog_ = {}

                        def tile_order(d):
                            return list(range(NT)) if d == 0 else [1, 0] + list(range(NT - 1, 1, -1))

                        def prep_gen(d, h):
                            dh = d * 4 + h
                            wk = WK[d]
                            bA, bB, bC = PSB[3 * d], PSB[3 * d + 1], PSB[3 * d + 2]
                            for n_, t in enumerate(tile_order(d)):
                                while n_ - prog_[('scan', d)] >= 2:
                                    yield
                                hs = HS[d][n_ % 2]
                                cs = col0(t)
                                kT_t, qT_t, vT_t = kT[:, cs:cs + 128], qT[:, cs:cs + 128], vT[:, cs:cs + 128]
                                sc = lambda a: a[:, t, dh:dh + 1]
                                P.copy('pool', wk["gb"][:], sc(gtm).to_broadcast([128, 128]))
                                P.mm(bA[:, 0:130], wk["gb"][:], trix[:, d, :])
                                yield
                                P.act(wk["eG"][:], bA[:, 0:128], AF.Exp)
                                P.act(hs["dcs"][:], bA[:, 128:130], AF.Exp)
                                P.ts('dve', wk["E1"][:], bA[:, 0:128], sc(Gc), ALU.subtract, 0.0, ALU.max)
                                P.ts('dve', wk["E2"][:], bA[:, 0:128], sc(Gc), ALU.subtract, 0.0, ALU.min)
                                yield
                                P.act(wk["E1"][:], wk["E1"][:], AF.Exp, scale=-1.0)
                                P.act(wk["E2"][:], wk["E2"][:], AF.Exp)
                                P.tt('pool', wk["E1"][:], wk["E1"][:], masks[:, d, :], ALU.mult)
                                P.tt('pool', wk["E2"][:], wk["E2"][:], masks[:, 2 + d, :], ALU.mult)
                                P.mm(bB[:, 0:128], kT_t, kT_t)
                                P.mm(bB[:, 128:256], kT_t, qT_t)
                                yield
                                L, LT, TT = wk["Pa"], wk["Qa"], wk["Ta"]
                                P.stt('dve', L[:], bB[:, 0:128], sc(beta), wk["E1"][:], ALU.mult, ALU.mult)
                                P.tt('dve', hs["intraT"][:], bB[:, 128:256], wk["E2"][:], ALU.mult)
                                P.tr(bB[:, 256:384], L[:], ident[:])
                                yield
                                P.copy('act', LT[:], bB[:, 256:384])
                                P.tt('dve', TT[:], ident[:], bB[:, 256:384], ALU.subtract)
                                Pc, Qc, TTc = L, LT, TT
                                Pn, Qn, TTn = wk["Pb"], wk["Qb"], wk["Tb"]
                                for kk in range(1, 6):
                                    P.mm(bA[:, 0:128], Qc[:], Pc[:])
                                    if kk < 5:
                                        P.mm(bA[:, 128:256], Pc[:], Qc[:])
                                    yield
                                    P.copy('act', Pn[:], bA[:, 0:128])
                                    if kk < 5:
                                        P.copy('dve', Qn[:], bA[:, 128:256])
                                    P.mm(bA[:, 256:384], Pn[:], TTc[:])
                                    yield
                                    P.tt('dve', TTn[:], TTc[:], bA[:, 256:384], ALU.add)
                                    Pc, Pn = Pn, Pc
                                    Qc, Qn = Qn, Qc
                                    TTc, TTn = TTn, TTc
                                P.tr(bB[:, 0:128], kT_t, ident[:])
                                P.tr(bB[:, 128:256], vT_t, ident[:])
                                yield
                                P.ts('dve', wk["kb"][:], bB[:, 0:128], sc(bk), ALU.mult)
                                P.ts('dve', hs["ktail"][:], bB[:, 0:128], sc(et), ALU.mult)
                                P.act(wk["vb"][:], bB[:, 128:256], AF.Copy, scale=sc(beta))
                                P.mm(bC[:, 0:128], TTc[:], wk["vb"][:])
                                P.mm(bC[:, 128:256], wk["kb"][:], TTc[:])
                                yield
                                P.copy('act', hs["u"][:], bC[:, 0:128])
                                P.copy('dve', hs["wT"][:], bC[:, 128:256])
                                P.tt('pool', hs["qdT"][:], qT_t, wk["eG"][:], ALU.mult)
                                prog_[('prep', d)] = n_ + 1
                                yield

                        def scan_gen(d, h):
                            wk = WK[d]
                            bS = R32d[d]
                            S = wk["S"]
                            P.memset('pool', S[:], 0.0)
                            P.memset('pool', wk["vnew"][:], 0.0)
                            for n_, t in enumerate(tile_order(d)):
                                while prog_[('prep', d)] <= n_:
                                    yield
                                hs = HS[d][n_ % 2]
                                need_o = upd or t >= 2
                                for c in ((0, 1) if d == 0 else (1, 0)):
                                    R = slice(64 * c, 64 * c + 64)
                                    P.mm(bS[:, 0:128], hs["wT"][:], S[:])
                                    yield
                                    P.tt('dve', wk["vnew"][R, :], hs["u"][R, :], bS[R, 0:128], ALU.subtract)
                                    if need_o:
                                        P.mm(bS[:, 128:256], hs["qdT"][:], S[:], start=True, stop=False)
                                        P.mm(bS[:, 128:256], hs["intraT"][:], wk["vnew"][:], start=False, stop=True)
                                    P.mm(bS[:, 256:384], hs["ktail"][R, :], wk["vnew"][R, :])
                                    yield
                                    if need_o:
                                        P.tt('dve', obuf[R, t, :], obuf[R, t, :], bS[R, 128:256], ALU.add)
                                    P.stt('dve', S[:], S[:], hs["dcs"][:, c:c + 1], bS[:, 256:384], ALU.mult, ALU.add)
                                prog_[('scan', d)] = n_ + 1
                                yield

                        heads = opts.get('heads', range(4))
                        for h in heads:
                            P.memset('pool', pre[:], 0.0)
                            for qi, dst in enumerate((qT, kT, vT)):
                                wpc_ = wpc[qi % 2]
                                load_win(l, qi * 512 + h * 128, 128, wpc_[:])
                                for bi, (tok0, ntok, cst) in enumerate(seqblocks):
                                    ps = PSB[bi % 2]
                                    for k in range(8):
                                        P.mm(ps[:, 0:ntok], wpc_[:, k, :], xnT[:, k, tok0:tok0 + ntok],
                                             start=(k == 0), stop=(k == 7))
                                    P.copy('act' if bi % 2 else 'dve', pre[:, cst:cst + ntok], ps[:, 0:ntok])
                                cwv = lambda j: cw[:, qi * 4 + h, j:j + 1]
                                P.ts('dve', dst[:, 2:2 + CW], pre[:, 0:CW], cwv(0), ALU.mult)
                                for j in range(1, 4):
                                    P.stt('dve', dst[:, 2:2 + CW], pre[:, j:j + CW], cwv(j),
                                          dst[:, 2:2 + CW], ALU.mult, ALU.add)
                                P.act(dst[:, 2:2 + CW], dst[:, 2:2 + CW], AF.Silu)
                                if qi < 2:
                                    for cb in range(5):
                                        c0 = 2 + cb * 512
                                        n = min(512, 2 + CW - c0)
                                        ps = PSB[2 + cb % 2]
                                        P.tt('pool', sq[:, 0:n], dst[:, c0:c0 + n], dst[:, c0:c0 + n], ALU.mult)
                                        P.mm(ps[:, 0:n], ones[:], sq[:, 0:n])
                                        P.ts('dve', rn[:, 0:n], ps[:, 0:n], 1e-6, ALU.add)
                                        P.act(rn[:, 0:n], rn[:, 0:n], AF.Ln)
                                        P.act(rn[:, 0:n], rn[:, 0:n], AF.Exp, scale=-0.5)
                                        if qi == 0:
                                            P.stt('dve', dst[:, c0:c0 + n], dst[:, c0:c0 + n], 128.0 ** -0.5, rn[:, 0:n],
                                                  ALU.mult, ALU.mult)
                                        else:
                                            P.tt('dve', dst[:, c0:c0 + n], dst[:, c0:c0 + n], rn[:, 0:n], ALU.mult)
                            if h == 0:
                                tap(f"L{l}_qT", qT[:, 261:261 + 512], [128, 512])
                                tap(f"L{l}_kT", kT[:, 261:261 + 512], [128, 512])
                                tap(f"L{l}_vT", vT[:, 2:2 + 256], [128, 256])
                            P.memset('pool', pre[:], 0.0)
                            for d_ in range(2):
                                prog_[('prep', d_)] = 0
                                prog_[('scan', d_)] = 0
                            gens = [prep_gen(0, h), prep_gen(1, h), scan_gen(0, h), scan_gen(1, h)]
                            alive = [True] * 4
                            while any(alive):
                                for gi, g in enumerate(gens):
                                    if alive[gi]:
                                        try:
                                            next(g)
                                        except StopIteration:
                                            alive[gi] = False
                            if h == 0:
                                tap(f"L{l}_o", pre[:, 256:256 + 512], [128, 512])
                            load_win(l, COL_ALPHA + h * 128, 128, wz[:])
                            P.load('pool', woh[:], D['w_out'][l][h * 128:(h + 1) * 128, :])
                            for t in tiles_upd:
                                P.act(fin["junk"][:], obuf[:, t, :], AF.Square, accum_out=oss[:, t:t + 1])
                            P.ts('dve', ors[:], oss[:], 1.0 / 128, ALU.mult, 1e-6, ALU.add)
                            P.act(ors[:], ors[:], AF.Ln)
                            P.act(ors[:], ors[:], AF.Exp, scale=-0.5)
                            for t in tiles_upd:
                                psZ = PSB[t % 2]
                                for k in range(8):
                                    P.mm(psZ[:, 0:128], xnT[:, k, t * 128:(t + 1) * 128], wz[:, k, :],
                                         start=(k == 0), stop=(k == 7))
                                P.act(fin["sz"][:], psZ[:, 0:128], AF.Silu)
                                P.stt('dve', fin["y1"][:], obuf[:, t, :], ors[:, t:t + 1], dnw[:], ALU.mult, ALU.mult)
                                P.tt('dve', y2[t % 2][:], fin["y1"][:], fin["sz"][:], ALU.mult)
                                P.tr(PTB[t % 2][:, 0:128], y2[t % 2][:], identb[:])
                                P.copy('act', mixT[t % 2][:], PTB[t % 2][:, 0:128])
                                psO = [PSB[2 + 2 * (t % 2)], PSB[3 + 2 * (t % 2)]]
                                for n in range(2):
                                    P.mm(psO[n][:], mixT[t % 2][:], woh[:, n * 512:(n + 1) * 512])
                                residual_add(t, [psO[0][:], psO[1][:]], g1rep[1 if t < 2 else 0])
                        P.barrier()
                    if check_stop(f"dn{l}"):
                        break

            with ExitStack() as tmp:
                EUV = EB[l]
                a2 = sb(tmp, "a2", [128, 1024])
                s2 = sb(tmp, "s2", [128, 1024])
                g2 = sb(tmp, "g2", [128, 1024])
                n2 = sb(tmp, "n2", [128, 1024])
                P.load('sp', n2[:], D['norm2_w'][l:l + 1, :].to_broadcast([128, 1024]))

                def load_mod2(which):
                    with ExitStack() as t2:
                        wm_ = sb(t2, "wm2", [128, 8, 256])
                        brow_ = sb(t2, "brow2", [128, 256])
                        cb1 = sb(t2, "cbc2", [128, 8, 128])
                        P.copy('pool', cb1[:], csil[:, :, which:which + 1].to_broadcast([128, 8, 128]))
                        wv = D['w_mod'][l].rearrange("(k p) n -> p k n", p=128)
                        i = 0
                        for dst, c0 in ((s2, 3072), (a2, 4096), (g2, 5120)):
                            for cb in range(4):
                                cc = c0 + cb * 256
                                psb = PSB[i % 2][:, 0:256]
                                P.load('sp', wm_[:], wv[:, :, cc:cc + 256])
                                P.load('sp', brow_[:], D['b_mod'][l:l + 1, cc:cc + 256].to_broadcast([128, 256]))
                                for k in range(8):
                                    P.mm(psb, cb1[:, k, :], wm_[:, k, :], start=(k == 0), stop=(k == 7))
                                P.tt('dve', dst[:, cb * 256:(cb + 1) * 256], psb, brow_[:], ALU.add)
                                i += 1
                        P.stt('dve', a2[:], a2[:], 1.0, n2[:], ALU.add, ALU.mult)
                        P.barrier()

                wq = sb(tmp, "wq", [128, 8, 2048], BF16)
                wqv = D['w_query'][l].rearrange("(k p) n -> p k n", p=128)
                for i in range(4):
                    P.load('pool', wq[:, :, i * 512:(i + 1) * 512], wqv[:, :, i * 512:(i + 1) * 512])
                keys = sb(tmp, "keys", [128, 16, 128], BF16)
                P.load('pool', keys[:], D['keysT'][l])
                zc = sb(tmp, "zc", [128, 255])
                P.load('sp', zc[:], D['zc'])
                iota3 = sb(tmp, "iota3", [128, 16, 16])
                P.load('sp', iota3[:], D['iota16'].unsqueeze(1).to_broadcast([128, 16, 16]))
                NB = 9
                GBH = {}
                junkp = [sb(tmp, f"junkp{i}", [128, 1024], BF16) for i in range(2)]
                hiS = [sb(tmp, f"hi{i}", [128, 1024], BF16) for i in range(2)]
                idxTS = [sb(tmp, f"idxT{i}", [128, 128], I32) for i in range(2)]
                gTS = [sb(tmp, f"gT{i}", [128, 128]) for i in range(2)]
                hxT = sb(tmp, "hxT", [128, 8, 128], BF16)
                qTs = sb(tmp, "qTs", [128, 16, 128], BF16)
                sc4 = [sb(tmp, f"sc4_{i}", [128, 512]) for i in range(2)]
                wk128 = sb(tmp, "wk128", [128, 128])
                wk256 = sb(tmp, "wk256", [128, 256])
                cand = sb(tmp, "cand", [128, 16, 16])
                m16 = sb(tmp, "m16", [128, 16, 16])
                i16u = sb(tmp, "i16u", [128, 16, 16], U32)
                i16f = sb(tmp, "i16f", [128, 16, 16])
                best = sb(tmp, "best", [128, 8, 16])
                posu = sb(tmp, "posu", [128, 8, 16], U32)
                pa_u = sb(tmp, "pa_u", [128, 8, 16], U32)
                pb_u = sb(tmp, "pb_u", [128, 8, 16], U32)
                pa_f = sb(tmp, "pa_f", [128, 8, 16])
                pb_f = sb(tmp, "pb_f", [128, 8, 16])
                oh = sb(tmp, "oh", [128, 4, 16, 16])
                sel1 = sb(tmp, "sel1", [128, 8, 16])
                sel2 = sb(tmp, "sel2", [128, 8, 16])
                idxf = sb(tmp, "idxf", [128, 8, 16])
                gate = sb(tmp, "gate", [128, 8, 16])
                gsum = sb(tmp, "gsum", [128, 8])
                actr = sb(tmp, "actr", [128, 128])
                actg = sb(tmp, "actg", [128, 128])
                P.gran[actr.name] = 1
                P.gran[actg.name] = 1
                lh = [sb(tmp, f"lh{i}", [128, 128], BF16) for i in range(2)]
                rtmp = sb(tmp, "rtmp", [128, 512])
                rstd_all(tiles_upd, junkp[0])
                hx32 = tmpr[0]
                ntok_lim = opts.get('peer_ntok', 128)
                R32 = [PTB[i][:].bitcast(F32) for i in range(2)]

                def routing(t, slot):
                    hi, idxT, gT = hiS[slot], idxTS[slot], gTS[slot]
                    P.stt('dve', hx32[:], XR[t][:], rs[:, t:t + 1], a2[:], ALU.mult, ALU.mult)
                    P.tt('dve', hx32[:], hx32[:], s2[:], ALU.add)
                    yield
                    P.copy('act', hi[:], hx32[:])
                    if t == 2:
                        tap(f"L{l}_hx2", hx32[:], [128, 1024])
                    yield
                    for half in range(2):
                        for kk in range(4):
                            k = half * 4 + kk
                            P.tr(PTB[half][:, kk * 256:kk * 256 + 128], hi[:, k * 128:(k + 1) * 128], identb[:])
                        yield
                        P.copy('act', hxT[:, half * 4:(half + 1) * 4, :],
                               PTB[half][:].rearrange("p (a b) -> p a b", b=256)[:, :, 0:128])
                        yield
                    for hp in range(16):
                        bank = R32[hp % 2]
                        for k in range(8):
                            P.mm(bank[:, 0:128], wq[:, k, hp * 128:(hp + 1) * 128], hxT[:, k, :],
                                 start=(k == 0), stop=(k == 7))
                            if k % 4 == 3:
                                yield
                        P.copy('act', qTs[:, hp, :], bank[:, 0:128])
                        yield
                    for b4 in range(4):
                        bank = R32[b4 % 2]
                        for q4 in range(4):
                            hp = b4 * 4 + q4
                            P.mm(bank[:, q4 * 128:(q4 + 1) * 128], qTs[:, hp, :], keys[:, hp, :])
                        yield
                        sc = sc4[b4 % 2]
                        P.copy('act', sc[:], bank[:, :])
                        yield
                        for q4 in range(4):
                            hp = b4 * 4 + q4
                            v = sc[:, q4 * 128:(q4 + 1) * 128]
                            P.op('dve', lambda E, hp=hp, v=v: E.max(out=m16[:, hp, 0:8], in_=v), r=[sc[:]], w=[m16[:]])
                            P.op('dve', lambda E, hp=hp, v=v: E.max_index(out=i16u[:, hp, 0:8], in_max=m16[:, hp, 0:8],
                                                                          in_values=v), r=[sc[:], m16[:]], w=[i16u[:]])
                            yield
                            P.op('dve', lambda E, hp=hp, v=v: E.match_replace(out=wk128[:], in_to_replace=m16[:, hp, 0:8],
                                                                              in_values=v, imm_value=-1e30),
                                 r=[sc[:], m16[:]], w=[wk128[:]])
                            P.op('dve', lambda E, hp=hp: E.max(out=m16[:, hp, 8:16], in_=wk128[:]), r=[wk128[:]], w=[m16[:]])
                            yield
                            P.op('dve', lambda E, hp=hp: E.max_index(out=i16u[:, hp, 8:16], in_max=m16[:, hp, 8:16],
                                                                     in_values=wk128[:]), r=[wk128[:], m16[:]], w=[i16u[:]])
                            yield
                    P.copy('dve', i16f[:], i16u[:])
                    yield
                    for h in range(8):
                        P.tt('dve', cand[:], m16[:, 2 * h, :].unsqueeze(2).to_broadcast([128, 16, 16]),
                             m16[:, 2 * h + 1, :].unsqueeze(1).to_broadcast([128, 16, 16]), ALU.add)
                        cf = cand[:].rearrange("p a b -> p (a b)")
                        yield
                        P.op('dve', lambda E, h=h, cf=cf: E.max(out=best[:, h, 0:8], in_=cf), r=[cand[:]], w=[best[:]])
                        P.op('dve', lambda E, h=h, cf=cf: E.max_index(out=posu[:, h, 0:8], in_max=best[:, h, 0:8],
                                                                      in_values=cf), r=[cand[:], best[:]], w=[posu[:]])
                        yield
                        P.op('dve', lambda E, h=h, cf=cf: E.match_replace(out=wk256[:], in_to_replace=best[:, h, 0:8],
                                                                          in_values=cf, imm_value=-1e30),
                             r=[cand[:], best[:]], w=[wk256[:]])
                        P.op('dve', lambda E, h=h: E.max(out=best[:, h, 8:16], in_=wk256[:]), r=[wk256[:]], w=[best[:]])
                        yield
                        P.op('dve', lambda E, h=h: E.max_index(out=posu[:, h, 8:16], in_max=best[:, h, 8:16],
                                                               in_values=wk256[:]), r=[wk256[:], best[:]], w=[posu[:]])
                        yield
                    P.op('dve', lambda E: E.tensor_single_scalar(pa_u[:], posu[:], 4, ALU.logical_shift_right),
                         r=[posu[:]], w=[pa_u[:]])
                    P.op('dve', lambda E: E.tensor_single_scalar(pb_u[:], posu[:], 15, ALU.bitwise_and),
                         r=[posu[:]], w=[pb_u[:]])
                    yield
                    P.copy('dve', pa_f[:], pa_u[:])
                    P.copy('dve', pb_f[:], pb_u[:])
                    yield
                    i4 = i16f[:].rearrange("p (h two) k -> p h two k", two=2)
                    io4 = iota3[:].unsqueeze(1).to_broadcast([128, 4, 16, 16])
                    for pf, half, dst in ((pa_f, 0, sel1), (pb_f, 1, sel2)):
                        for hh in range(2):
                            hs = slice(hh * 4, hh * 4 + 4)
                            P.tt('dve', oh[:], pf[:, hs, :].unsqueeze(3).to_broadcast([128, 4, 16, 16]), io4, ALU.is_equal)
                            yield
                            P.tt('dve', oh[:], oh[:], i4[:, hs, half, :].unsqueeze(2).to_broadcast([128, 4, 16, 16]),
                                 ALU.mult)
                            yield
                            P.op('dve', lambda E, dst=dst, hs=hs: E.reduce_sum(out=dst[:, hs, :], in_=oh[:], axis=AX.X),
                                 r=[oh[:]], w=[dst[:]])
                            yield
                    P.stt('dve', idxf[:], sel1[:], 128.0, sel2[:], ALU.mult, ALU.add)
                    P.tt('dve', gate[:], best[:], best[:, :, 0:1].to_broadcast([128, 8, 16]), ALU.subtract)
                    yield
                    P.act(gate[:], gate[:], AF.Exp)
                    yield
                    P.op('dve', lambda E: E.reduce_sum(out=gsum[:], in_=gate[:], axis=AX.X), r=[gate[:]], w=[gsum[:]])
                    P.op('dve', lambda E: E.reciprocal(out=gsum[:], in_=gsum[:]), r=[gsum[:]], w=[gsum[:]])
                    yield
                    P.tt('dve', gate[:], gate[:], gsum[:].unsqueeze(2).to_broadcast([128, 8, 16]), ALU.mult)
                    if t == 2:
                        tap(f"L{l}_idx", idxf[:].rearrange("p a b -> p (a b)"), [128, 128])
                        tap(f"L{l}_gate", gate[:].rearrange("p a b -> p (a b)"), [128, 128])
                    yield
                    P.tr(R32[0][:, 0:128], idxf[:].rearrange("p a b -> p (a b)"), ident[:])
                    P.tr(R32[1][:, 0:128], gate[:].rearrange("p a b -> p (a b)"), ident[:])
                    yield
                    P.copy('act', idxT[:], R32[0][:, 0:128])
                    P.copy('act', gT[:], R32[1][:, 0:128])
                    yield

                def token_loop(t, slot, bg):
                    hi, idxT, gT = hiS[slot], idxTS[slot], gTS[slot]
                    GB = GBH['gb']
                    P.memset('pool', actr[:], 0.0)
                    ops_ = PS2[2]
                    LOOK = 5
                    n_tok = ntok_lim

                    def issue_dma(j):
                        g = GB[j % NB]
                        P.dma('pool', lambda E, g=g, j=j: E.indirect_dma_start(
                            out=g[:], out_offset=None, in_=EUV,
                            in_offset=bass.IndirectOffsetOnAxis(ap=idxT[:, j:j + 1], axis=0)),
                            r=[idxT[:], EUV], w=[g[:]])

                    def hbmm(j):
                        hb = PS2[j % 2]
                        sel = identb[:, j:j + 1].to_broadcast([128, 128])
                        for n in range(2):
                            P.mm(hb[:, n * 512:(n + 1) * 512], sel, hi[:, n * 512:(n + 1) * 512])

                    for j in range(min(LOOK, n_tok)):
                        issue_dma(j)
                    hbmm(0)
                    for i in range(n_tok + 3):
                        if i + LOOK < n_tok:
                            issue_dma(i + LOOK)
                        if i + 1 < n_tok:
                            hbmm(i + 1)
                        if i < n_tok:
                            P.stt('dve', junkp[i % 2][:], GB[i % NB][:, 0:1024], 1.0, PS2[i % 2][:], ALU.mult, ALU.mult,
                                  accum_out=actr[:, i:i + 1])
                        j1 = i - 1
                        if 0 <= j1 < n_tok:
                            P.act(actg[:, j1:j1 + 1], actr[:, j1:j1 + 1], AF.Gelu)
                        j2 = i - 2
                        if 0 <= j2 < n_tok:
                            P.ts('dve', lh[j2 % 2][:], zc[:, 127 - j2:255 - j2], actg[:, j2:j2 + 1], ALU.mult,
                                 gT[:, j2:j2 + 1], ALU.mult)
                        j3 = i - 3
                        if 0 <= j3 < n_tok:
                            g = GB[j3 % NB]
                            for n in range(2):
                                P.mm(ops_[:, n * 512:(n + 1) * 512], lh[j3 % 2][:],
                                     g[:, 1024 + n * 512:1024 + (n + 1) * 512],
                                     start=(j3 == 0), stop=(j3 == n_tok - 1))
                        if bg is not None and i >= 4:
                            for _ in range(opts.get('bg_steps', 3)):
                                try:
                                    next(bg)
                                except StopIteration:
                                    bg = None
                                    break
                    if bg is not None:
                        for _ in bg:
                            pass
                    if t == 2 and f"L{l}_peer" in taps_req:
                        po = sb(GBH['tg'], "peer_o", [128, 1024])
                        P.copy('dve', po[:], ops_[:])
                        tap(f"L{l}_peer", po[:], [128, 1024])
                    for n in range(2):
                        P.tt('dve', rtmp[:], ops_[:, n * 512:(n + 1) * 512], g2[:, n * 512:(n + 1) * 512], ALU.mult)
                        P.tt('pool', XR[t][:, n * 512:(n + 1) * 512], XR[t][:, n * 512:(n + 1) * 512], rtmp[:], ALU.add)

                ptiles = opts.get('peer_tiles', None)
                groups = ([(1, (0, 1))] if upd else []) + [(0, tuple(range(2, NT)))]
                for which, tl in groups:
                    tl = [t for t in tl if ptiles is None or t in ptiles]
                    if not tl:
                        continue
                    load_mod2(which)
                    with ExitStack() as tg:
                        GBH['tg'] = tg
                        GBH['gb'] = [sb(tg, f"gb{i}", [128, 2048], BF16) for i in range(NB)]
                        for _ in routing(tl[0], 0):
                            pass
                        for n_, t in enumerate(tl):
                            nxt = routing(tl[n_ + 1], (n_ + 1) % 2) if n_ + 1 < len(tl) else None
                            token_loop(t, n_ % 2, nxt)
                        P.barrier()
            if check_stop(f"peer{l}"):
                break

        if not done['stop']:
            with ExitStack() as tmp:
                junk = sb(tmp, "junkf", [128, 1024], BF16)
                fnw = sb(tmp, "fnw", [128, 1024])
                ot = [sb(tmp, f"ot{i}", [128, 1024]) for i in range(2)]
                P.load('sp', fnw[:], D['final_norm_w'].to_broadcast([128, 1024]))
                rstd_all(list(range(2, NT)), junk)
                for t in range(2, NT):
                    o_ = ot[t % 2]
                    P.stt('dve', o_[:], XR[t][:], rs[:, t:t + 1], fnw[:], ALU.mult, ALU.mult)
                    P.load('sp', out_d[(t - 2) * 128:(t - 1) * 128, :], o_[:])
        else:
            for t in range(2, NT):
                P.load('sp', out_d[(t - 2) * 128:(t - 1) * 128, :], XR[t][:])
        if 'z' in taps_req:
            for t in range(2):
                tap(f"z{t}", XR[t][:], [128, 1024], force=True)
        P.barrier()
        print("instr", P.ninstr, flush=True)
    return nc, tap_d


_CACHE = {}


def kernel(**inputs):
    inp = {k: np.asarray(v) for k, v in inputs.items()}
    if 'nc' not in _CACHE:
        _CACHE['nc'] = build({})[0]
    nc = _CACHE['nc']
    sh = prep_shared(inp)
    n = 8
    in_maps = []
    for b in range(n):
        cv = np.stack([inp['c'][b].reshape(8, 128).T, inp['c_ctx'].reshape(8, 128).T], axis=-1)
        m = dict(sh)
        m['x'] = np.ascontiguousarray(inp['x'][b], dtype=np.float32)
        m['ctx'] = np.ascontiguousarray(inp['ctx'][b], dtype=np.float32)
        m['cvec'] = np.ascontiguousarray(cv, dtype=np.float32)
        in_maps.append(m)
    res = run_bass_kernel_spmd(nc, in_maps, core_ids=list(range(n)))
    return np.stack([np.asarray(res.results[b]['out'], dtype=np.float32) for b in range(n)], axis=0)
```
